# Optimizing a Trainium2 kernel written in Bass

```python
import jax, jax.numpy as jnp
from jax import lax
import numpy as np

D_MODEL = 1024
BATCH = 32
SEQ = 2048
DEPTH = 1

GRID_W = 64
WIN_R = 8
WIN_C = 16
NA_HEADS = 8
NA_HEAD_DIM = 64
NA_WIDTH = NA_HEADS * NA_HEAD_DIM
RET_HEADS = 4
RET_HEAD_DIM = 128
RET_WIDTH = RET_HEADS * RET_HEAD_DIM
RET_CHUNK = 128
ROPE_BASE = 10000.0
D_MIX = NA_WIDTH + RET_WIDTH
D_IN_PROJ = 3 * NA_WIDTH + 4 * RET_WIDTH
N_EXPERTS = 256
TOP_K = 8
N_GROUPS = 8
TOPK_GROUPS = 4
D_EXPERT = 256
D_SHARED = 256
ROUTED_SCALE = 2.5
EXPERT_BLOCK = 128
BLOCKS_PER_STEP = 32
LN_EPS = 1e-5
GN_EPS = 1e-6
DEEPNORM_ALPHA = (2.0 * DEPTH) ** 0.25
DEEPNORM_BETA = (8.0 * DEPTH) ** -0.25

kernel_name = 'hybrid_natten_retnet_moe_block'


def _layer_norm(x, g, b):
    xf = x.astype(jnp.float32)
    mu = jnp.mean(xf, -1, keepdims=True)
    var = jnp.mean(jnp.square(xf - mu), -1, keepdims=True)
    y = (xf - mu) * lax.rsqrt(var + LN_EPS) * g.astype(jnp.float32) + b.astype(jnp.float32)
    return y.astype(x.dtype)


def _rope(t, cos, sin):
    half = t.shape[-1] // 2
    t1, t2 = t[..., :half], t[..., half:]
    return jnp.concatenate([t1 * cos - t2 * sin, t2 * cos + t1 * sin], axis=-1).astype(t.dtype)


def _neighbourhood_attention(q, k, v, rpb, rows):
    b, s, _ = q.shape
    w, h, d = GRID_W, NA_HEADS, NA_HEAD_DIM
    kr = min(WIN_R, rows)
    grid = lambda t: t.reshape(b, rows, w, h, d)
    q_rows = grid(q * d ** -0.5).transpose(1, 0, 3, 2, 4)
    k_grid = grid(k).transpose(0, 3, 1, 2, 4)
    v_grid = grid(v).transpose(0, 3, 1, 2, 4)
    cq = jnp.arange(w)
    cs = jnp.clip(cq - WIN_C // 2, 0, w - WIN_C)
    ck = jnp.arange(w)
    col_in = (ck[None, :] >= cs[:, None]) & (ck[None, :] < cs[:, None] + WIN_C)
    dc_idx = jnp.clip(ck[None, :] - cq[:, None] + WIN_C - 1, 0, 2 * WIN_C - 2)
    rpb_cols = rpb[:, :, dc_idx]

    def row_block(args):
        r, q_row = args
        rs = jnp.clip(r - kr // 2, 0, rows - kr)
        k_blk = lax.dynamic_slice_in_dim(k_grid, rs, kr, axis=2)
        v_blk = lax.dynamic_slice_in_dim(v_grid, rs, kr, axis=2)
        dr_idx = rs + jnp.arange(kr) - r + WIN_R - 1
        bias = rpb_cols[:, dr_idx].transpose(0, 2, 1, 3)
        sc = jnp.einsum('bhqd,bhrkd->bhqrk', q_row, k_blk).astype(jnp.float32)
        sc = sc + bias.astype(jnp.float32)[None]
        sc = jnp.where(col_in[:, None, :], sc, -jnp.inf)
        p = jax.nn.softmax(sc, axis=(-2, -1)).astype(v_blk.dtype)
        return jnp.einsum('bhqrk,bhrkd->bhqd', p, v_blk)

    out = lax.map(row_block, (jnp.arange(rows, dtype=jnp.int32), q_rows))
    return out.transpose(1, 0, 3, 2, 4).reshape(b, s, h * d)


def _retention_chunkwise(q, k, v, log_gamma, strict):
    b, h, s, dh = q.shape
    n = s // RET_CHUNK
    qc = q.reshape(b, h, n, RET_CHUNK, dh)
    kc = k.reshape(b, h, n, RET_CHUNK, dh)
    vc = v.reshape(b, h, n, RET_CHUNK, dh)
    lg = log_gamma.astype(jnp.float32)[:, None]
    i = jnp.arange(RET_CHUNK, dtype=jnp.float32)
    diff = i[:, None] - i[None, :]
    mask = (diff > 0) if strict else (diff >= 0)
    decay_in = jnp.where(mask, jnp.exp(jnp.maximum(diff, 0.0) * lg[:, :, None]), 0.0)
    sc = jnp.einsum('bhncd,bhnmd->bhncm', qc, kc) * decay_in[None, :, None]
    intra = jnp.einsum('bhncm,bhnme->bhnce', sc, vc)
    k_decay = jnp.exp((RET_CHUNK - 1 - i)[None, :] * lg)
    q_decay = jnp.exp((i + 1)[None, :] * lg)
    kv = jnp.einsum('bhncd,bhnce->nbhde', kc * k_decay[None, :, None, :, None], vc)
    chunk_decay = jnp.exp(RET_CHUNK * lg)[None, :, :, None]

    def step(state, kv_n):
        return chunk_decay * state + kv_n, state

    _, r_prev = lax.scan(step, jnp.zeros_like(kv[0]), kv)
    cross = jnp.einsum('bhncd,nbhde->bhnce', qc * q_decay[None, :, None, :, None], r_prev)
    return (intra + cross).reshape(b, h, s, dh)


def _bidirectional_retention(q, k, v, g, log_decay, gn_g, cos, sin):
    b, s, _ = q.shape
    h, dh = RET_HEADS, RET_HEAD_DIM
    qh = _rope(q.reshape(b, s, h, dh), cos, sin).transpose(0, 2, 1, 3)
    kh = (_rope(k.reshape(b, s, h, dh), cos, sin) * dh ** -0.5).transpose(0, 2, 1, 3)
    vh = v.reshape(b, s, h, dh).transpose(0, 2, 1, 3)
    y_fwd = _retention_chunkwise(qh, kh, vh, log_decay[0], False)
    y_bwd = jnp.flip(_retention_chunkwise(jnp.flip(qh, 2), jnp.flip(kh, 2), jnp.flip(vh, 2),
                                          log_decay[1], True), 2)
    y = (y_fwd + y_bwd).astype(jnp.float32)
    mu = jnp.mean(y, -1, keepdims=True)
    var = jnp.mean(jnp.square(y - mu), -1, keepdims=True)
    yn = ((y - mu) * lax.rsqrt(var + GN_EPS)).transpose(0, 2, 1, 3).reshape(b, s, h * dh)
    yn = yn * gn_g.astype(jnp.float32)
    return (jax.nn.silu(g.astype(jnp.float32)) * yn).astype(g.dtype)


def _token_mixer(h, w_in, w_out, rpb, log_decay, gn_g, cos, sin, rows):
    proj = jnp.einsum('bsd,de->bse', h, w_in)
    cuts = [NA_WIDTH, 2 * NA_WIDTH, 3 * NA_WIDTH, 3 * NA_WIDTH + RET_WIDTH,
            3 * NA_WIDTH + 2 * RET_WIDTH, 3 * NA_WIDTH + 3 * RET_WIDTH]
    q_na, k_na, v_na, q_r, k_r, v_r, g_r = jnp.split(proj, cuts, axis=-1)
    y_na = _neighbourhood_attention(q_na, k_na, v_na, rpb, rows)
    y_r = _bidirectional_retention(q_r, k_r, v_r, g_r, log_decay, gn_g, cos, sin)
    return jnp.einsum('bse,ed->bsd', jnp.concatenate([y_na, y_r], axis=-1), w_out)


def _route(xf, w_router, router_bias):
    n = xf.shape[0]
    scores = jax.nn.sigmoid(jnp.einsum('nd,de->ne', xf, w_router).astype(jnp.float32))
    sel = scores + router_bias.astype(jnp.float32)[None]
    grp = sel.reshape(n, N_GROUPS, N_EXPERTS // N_GROUPS)
    group_score = lax.top_k(grp, 2)[0].sum(-1)
    _, gidx = lax.top_k(group_score, TOPK_GROUPS)
    gmask = jax.nn.one_hot(gidx, N_GROUPS, dtype=jnp.float32).sum(1) > 0
    masked = jnp.where(gmask[:, :, None], grp, -jnp.inf).reshape(n, N_EXPERTS)
    _, topk_idx = lax.top_k(masked, TOP_K)
    w = jnp.take_along_axis(scores, topk_idx, axis=1)
    w = w / jnp.sum(w, -1, keepdims=True) * ROUTED_SCALE
    return topk_idx.astype(jnp.int32), w


def _routed_experts(xf, topk_idx, topk_w, w_gate, w_up, w_down):
    n, d = xf.shape
    a = n * TOP_K
    flat_e = topk_idx.reshape(a)
    flat_w = topk_w.reshape(a)
    flat_tok = jnp.arange(a, dtype=jnp.int32) // TOP_K
    order = jnp.argsort(flat_e)
    e_sorted = flat_e[order]
    counts = jnp.bincount(flat_e, length=N_EXPERTS).astype(jnp.int32)
    starts = jnp.cumsum(counts) - counts
    padded = (counts + EXPERT_BLOCK - 1) // EXPERT_BLOCK * EXPERT_BLOCK
    pends = jnp.cumsum(padded)
    pstarts = pends - padded
    dest = pstarts[e_sorted] + (jnp.arange(a, dtype=jnp.int32) - starts[e_sorted])
    n_blocks = -(-(a + N_EXPERTS * (EXPERT_BLOCK - 1)) // EXPERT_BLOCK)
    n_blocks = -(-n_blocks // BLOCKS_PER_STEP) * BLOCKS_PER_STEP
    p = n_blocks * EXPERT_BLOCK
    slot_tok = jnp.full((p,), n, jnp.int32).at[dest].set(flat_tok[order])
    slot_w = jnp.zeros((p,), flat_w.dtype).at[dest].set(flat_w[order])
    block_e = jnp.searchsorted(pends, jnp.arange(n_blocks, dtype=jnp.int32) * EXPERT_BLOCK, side='right')
    block_e = jnp.minimum(block_e, N_EXPERTS - 1).astype(jnp.int32)
    n_steps = n_blocks // BLOCKS_PER_STEP
    slot_tok = slot_tok.reshape(n_steps, BLOCKS_PER_STEP, EXPERT_BLOCK)
    slot_w = slot_w.reshape(n_steps, BLOCKS_PER_STEP, EXPERT_BLOCK)
    block_e = block_e.reshape(n_steps, BLOCKS_PER_STEP)
    x_pad = jnp.concatenate([xf, jnp.zeros((1, d), xf.dtype)], axis=0)

    def step(acc, inp):
        tok, wt, e = inp
        xb = x_pad[tok]
        hg = jnp.einsum('sbd,sdf->sbf', xb, w_gate[e])
        hu = jnp.einsum('sbd,sdf->sbf', xb, w_up[e])
        y = jnp.einsum('sbf,sfd->sbd', jax.nn.silu(hg) * hu, w_down[e]) * wt[..., None]
        acc = acc.at[tok.reshape(-1)].add(y.reshape(-1, d).astype(acc.dtype))
        return acc, None

    acc, _ = lax.scan(step, jnp.zeros((n + 1, d), xf.dtype), (slot_tok, slot_w, block_e))
    return acc[:n]


def _moe_ffn(xf, w_router, router_bias, w_gate, w_up, w_down, ws_gate, ws_up, ws_down):
    shared = jnp.einsum('nf,fd->nd', jax.nn.silu(xf @ ws_gate) * (xf @ ws_up), ws_down)
    topk_idx, topk_w = _route(xf, w_router, router_bias)
    return shared + _routed_experts(xf, topk_idx, topk_w, w_gate, w_up, w_down)


def setup_inputs(seed: int = 0) -> dict:
    key = jax.random.key(seed)
    ks = jax.random.split(key, 24)
    L, D, E = DEPTH, D_MODEL, N_EXPERTS
    nrm = lambda k, shape, scale: jax.random.normal(k, shape, jnp.float32) * scale
    beta = DEEPNORM_BETA
    col_scale = np.concatenate([np.ones(2 * NA_WIDTH), np.full(NA_WIDTH, beta), np.ones(2 * RET_WIDTH),
                                np.full(RET_WIDTH, beta), np.ones(RET_WIDTH)]).astype(np.float32)
    base_decay = np.log(1.0 - 2.0 ** (-5.0 - np.arange(RET_HEADS))).astype(np.float32)
    ret_log_decay = jnp.asarray(base_decay)[None, None, :] * jnp.exp(nrm(ks[7], (L, 2, RET_HEADS), 0.1))
    return {
        'x': nrm(ks[0], (BATCH, SEQ, D), 1.0),
        'c': nrm(ks[1], (BATCH, D), 1.0),
        'w_ada': nrm(ks[2], (L, D, 6 * D), D ** -0.5),
        'b_ada': nrm(ks[3], (L, 6 * D), 0.02),
        'w_in': nrm(ks[4], (L, D, D_IN_PROJ), D ** -0.5) * jnp.asarray(col_scale),
        'w_out': nrm(ks[5], (L, D_MIX, D), beta * D_MIX ** -0.5),
        'na_rpb': nrm(ks[6], (L, NA_HEADS, 2 * WIN_R - 1, 2 * WIN_C - 1), 0.1),
        'ret_log_decay': ret_log_decay,
        'ret_gn_g': 1.0 + nrm(ks[8], (L, RET_WIDTH), 0.02),
        'ln1_g': 1.0 + nrm(ks[9], (L, D), 0.02),
        'ln1_b': nrm(ks[10], (L, D), 0.02),
        'ln2_g': 1.0 + nrm(ks[11], (L, D), 0.02),
        'ln2_b': nrm(ks[12], (L, D), 0.02),
        'w_router': nrm(ks[13], (L, D, E), D ** -0.5),
        'router_bias': nrm(ks[14], (L, E), 0.01),
        'w_gate': nrm(ks[15], (L, E, D, D_EXPERT), beta * D ** -0.5),
        'w_up': nrm(ks[16], (L, E, D, D_EXPERT), beta * D ** -0.5),
        'w_down': nrm(ks[17], (L, E, D_EXPERT, D), beta * D_EXPERT ** -0.5),
        'ws_gate': nrm(ks[18], (L, D, D_SHARED), beta * D ** -0.5),
        'ws_up': nrm(ks[19], (L, D, D_SHARED), beta * D ** -0.5),
        'ws_down': nrm(ks[20], (L, D_SHARED, D), beta * D_SHARED ** -0.5),
    }


def reference(x, c, w_ada, b_ada, w_in, w_out, na_rpb, ret_log_decay, ret_gn_g, ln1_g, ln1_b,
              ln2_g, ln2_b, w_router, router_bias, w_gate, w_up, w_down, ws_gate, ws_up, ws_down):
    b, s, d = x.shape
    rows = s // GRID_W
    t = jnp.arange(s, dtype=jnp.float32)
    inv_freq = ROPE_BASE ** (-jnp.arange(0, RET_HEAD_DIM, 2, dtype=jnp.float32) / RET_HEAD_DIM)
    ang = t[:, None] * inv_freq[None, :]
    cos, sin = jnp.cos(ang)[:, None, :], jnp.sin(ang)[:, None, :]
    cond = jax.nn.silu(c)
    for l in range(DEPTH):
        mod = jnp.einsum('bd,de->be', cond, w_ada[l]) + b_ada[l]
        shift_a, scale_a, gate_a, shift_f, scale_f, gate_f = [m[:, None, :] for m in jnp.split(mod, 6, axis=-1)]
        h = x * (1.0 + scale_a) + shift_a
        mix = _token_mixer(h, w_in[l], w_out[l], na_rpb[l], ret_log_decay[l], ret_gn_g[l], cos, sin, rows)
        x = _layer_norm(DEEPNORM_ALPHA * x + gate_a * mix, ln1_g[l], ln1_b[l])
        hf = (x * (1.0 + scale_f) + shift_f).reshape(b * s, d)
        ffn = _moe_ffn(hf, w_router[l], router_bias[l], w_gate[l], w_up[l], w_down[l],
                       ws_gate[l], ws_up[l], ws_down[l]).reshape(b, s, d)
        x = _layer_norm(DEEPNORM_ALPHA * x + gate_f * ffn, ln2_g[l], ln2_b[l])
    return x
```

```python
import numpy as np
import os
from contextlib import ExitStack
import concourse.bass as bass
import concourse.mybir as mybir
from concourse.bass_utils import run_bass_kernel_spmd

F32 = mybir.dt.float32
BF16 = mybir.dt.bfloat16
I32 = mybir.dt.int32
AF = mybir.ActivationFunctionType
ALU = mybir.AluOpType
AX = mybir.AxisListType
ds = bass.ds

NCORES = 8
D = 1024
S = 2048
NT = 16
E = 256
ALPHA = 2.0 ** 0.25
NBLK_PER_SEQ = 192


class Buf:
    def __init__(self, t, name):
        self.t = t
        self.name = name
        self.w = {}
        self.r = {}
        self.lane = None

    def __getitem__(self, k):
        return self.t[k]


class Sched:
    def __init__(self, nc, es):
        self.nc = nc
        self.es = es
        self.engs = {'pe': nc.tensor, 'act': nc.scalar, 'dve': nc.vector, 'pool': nc.gpsimd, 'sp': nc.sync}
        self.semh = {}
        self.cnt = {}
        self.waited = {k: {} for k in self.engs}
        for k in self.engs:
            self.semh[k] = es.enter_context(nc.semaphore('s_' + k))
            self.cnt[k] = 0
        self.nlanes = 0
        self.uid = 0
        self.scopes = [[]]
        self.es_stack = []

    def sb(self, name, shape, dt):
        self.uid += 1
        b = Buf(self.es.enter_context(self.nc.sbuf_tensor('sb%d_%s' % (self.uid, name), list(shape), dt)), name)
        self.scopes[-1].append(b)
        return b

    def push(self):
        self.es_stack.append(self.es)
        self.es = ExitStack()
        self.scopes.append([])

    def pop(self):
        bufs = self.scopes.pop()
        for eng in self.engs:
            self._deps(eng, bufs, bufs, skip_self=(eng == 'pe'))
        self.es.close()
        self.es = self.es_stack.pop()

    def _lane(self, b):
        if b.lane is None:
            b.lane = 'L%d_%s' % (self.nlanes, b.name)
            self.nlanes += 1
            self.semh[b.lane] = self.es.enter_context(self.nc.semaphore('s_' + b.lane))
            self.cnt[b.lane] = 0
        return b.lane

    def _deps(self, eng, reads, writes, skip_self=False, skip_sems=()):
        deps = {}
        for b in reads:
            for s, v in b.w.items():
                if deps.get(s, 0) < v:
                    deps[s] = v
        for b in writes:
            for dct in (b.w, b.r):
                for s, v in dct.items():
                    if deps.get(s, 0) < v:
                        deps[s] = v
        wd = self.waited[eng]
        for s, v in deps.items():
            if skip_self and s == eng:
                continue
            if s in skip_sems:
                continue
            if wd.get(s, 0) < v:
                self.engs[eng].wait_ge(self.semh[s], v)
                wd[s] = v

    def _mark(self, tok, reads, writes):
        s, v = tok
        for b in reads:
            if b.r.get(s, 0) < v:
                b.r[s] = v
        for b in writes:
            if b.w.get(s, 0) < v:
                b.w[s] = v

    def op(self, eng, fn, reads=(), writes=(), inc=True):
        self._deps(eng, reads, writes, skip_self=(eng == 'pe'))
        ins = fn(self.engs[eng])
        if inc:
            self.cnt[eng] += 1
            ins.then_inc(self.semh[eng], 1)
            tok = (eng, self.cnt[eng])
        else:
            tok = (eng, self.cnt[eng] + 1)
        self._mark(tok, reads, writes)
        return ins

    def flush(self, eng, scratch):
        self.op(eng, lambda e: e.tensor_copy(out=scratch[0:1, 0:1], in_=scratch[0:1, 1:2]), reads=(), writes=(scratch,))

    def dma(self, fn, reads, writes, q='sp'):
        dst = writes[0]
        lane = self._lane(dst)
        self._deps(q, reads, writes, skip_sems=(lane,))
        ins = fn(self.engs[q])
        self.cnt[lane] += 16
        ins.then_inc(self.semh[lane], 16)
        self._mark((lane, self.cnt[lane]), reads, writes)
        return ins

    def wait_everything(self, eng):
        for k, v in self.cnt.items():
            if v > 0 and self.waited[eng].get(k, 0) < v:
                self.engs[eng].wait_ge(self.semh[k], v)
                self.waited[eng][k] = v

    def wait_all(self, eng, bufs):
        self._deps(eng, bufs, bufs)


def nblk_for(nseq):
    n = -(-(nseq * S * 8 + E * 127) // 128)
    return n + (n % 2)


class _Stop(Exception):
    pass


def _na_classes():
    cls_of = {}
    classes = []
    for j in range(16):
        for kt in range(16):
            desc = []
            anyv = False
            for b in range(2):
                for a in range(2):
                    rq = 2 * j + a
                    rk = 2 * kt + b
                    rs = min(max(rq - 4, 0), 24)
                    v = rs <= rk < rs + 8
                    anyv = anyv or v
                    desc.append((v, rk - rq + 7 if v else -1))
            if not anyv:
                continue
            desc = tuple(desc)
            if desc not in classes:
                classes.append(desc)
            cls_of[(j, kt)] = classes.index(desc)
    return cls_of, classes


CLS_OF, CLASSES = _na_classes()
NCLS = len(CLASSES)


def _host_consts(rpb):
    c = {}
    c['ident'] = np.eye(128, dtype=np.float32)
    c['lstrict'] = np.triu(np.ones((128, 128), np.float32), 1)
    c['iota1'] = np.broadcast_to(np.arange(1, E + 1, dtype=np.float32)[None, :], (128, E)).copy()
    i = np.arange(128, dtype=np.float32)
    diff = i[None, :] - i[:, None]
    c['dpos'] = np.maximum(diff, 0.0).astype(np.float32)
    c['dneg'] = np.maximum(-diff, 0.0).astype(np.float32)
    c['maskf'] = (diff >= 0).astype(np.float32)
    c['maskb'] = (diff < 0).astype(np.float32)
    c['ifp1'] = np.broadcast_to((i + 1.0)[None, :], (128, 128)).copy().astype(np.float32)
    c['ifm'] = np.broadcast_to((128.0 - i)[None, :], (128, 128)).copy().astype(np.float32)
    c['pexp'] = np.stack([i, 127.0 - i, i + 1.0, 128.0 - i], axis=1).astype(np.float32)
    t = np.arange(S, dtype=np.float32)
    inv_freq = (np.float32(10000.0) ** (-np.arange(0, 128, 2, dtype=np.float32) / np.float32(128.0))).astype(np.float32)
    ang = (t[:, None] * inv_freq[None, :]).astype(np.float32)
    cos = np.cos(ang).astype(np.float32).reshape(NT, 128, 64).transpose(1, 0, 2)
    sin = np.sin(ang).astype(np.float32).reshape(NT, 128, 64).transpose(1, 0, 2)
    c['cos'] = np.ascontiguousarray(cos)
    c['sin'] = np.ascontiguousarray(sin)
    c['nsin'] = np.ascontiguousarray(-sin)
    ck = np.arange(64)[:, None]
    cq = np.arange(64)[None, :]
    dc = np.clip(ck - cq + 15, 0, 30)
    cs = np.clip(cq - 8, 0, 48)
    col_in = (ck >= cs) & (ck < cs + 16)
    bt = np.full((8, 128, NCLS, 128), -30000.0, np.float32)
    for ci, desc in enumerate(CLASSES):
        n = 0
        for b in range(2):
            for a in range(2):
                v, dr = desc[n]
                n += 1
                if not v:
                    continue
                blk = rpb[:, dr, :][:, dc]
                blk = np.where(col_in[None], blk, np.float32(-30000.0))
                bt[:, b * 64:(b + 1) * 64, ci, a * 64:(a + 1) * 64] = blk
    c['biasT'] = bt
    c['thrrow'] = None
    return c


def build_nc(nseq, debug=False, stop=None):
    try:
        return _build_nc(nseq, debug, stop)
    except _Stop as ex:
        return ex.args[0]


def _build_nc(nseq, debug=False, stop=None):
    nc = bass.Bass("TRN2", target_bir_lowering=False)
    NTOK = nseq * S
    NTT = nseq * NT
    NBLK = nblk_for(nseq)
    NSLOT = NBLK * 128

    def din(name, shape, dt=F32):
        return nc.dram_tensor(name, list(shape), dt, kind="ExternalInput").ap()

    x_d = din("x", [NTOK, D])
    cT_d = din("cT", [128, 8, nseq])
    wada_d = din("w_ada", [D, 6 * D])
    bada_d = din("b_ada", [1, 6 * D])
    win_d = din("w_in", [D, 3584])
    wout_d = din("w_out", [D, D])
    biasT_d = din("biasT", [8, 128, NCLS, 128])
    lg_d = din("lg", [1, 8])
    gng_d = din("gn_g", [1, 512])
    ln_d = din("ln", [4, D])
    wr_d = din("w_router", [D, E])
    rb_d = din("router_bias", [1, E])
    wcat_d = din("wcat", [E * 128, 6144])
    wsg_d = din("ws_gate", [D, 256])
    wsu_d = din("ws_up", [D, 256])
    wsd_d = din("ws_down", [256, D])
    ident_d = din("ident", [128, 128])
    lstrict_d = din("lstrict", [128, 128])
    iota1_d = din("iota1", [128, E])
    dpos_d = din("dpos", [128, 128])
    dneg_d = din("dneg", [128, 128])
    maskf_d = din("maskf", [128, 128])
    maskb_d = din("maskb", [128, 128])
    ifp1_d = din("ifp1", [128, 128])
    ifm_d = din("ifm", [128, 128])
    pexp_d = din("pexp", [128, 4])
    cos_d = din("cos", [128, NT, 64])
    sin_d = din("sin", [128, NT, 64])
    nsin_d = din("nsin", [128, NT, 64])
    thr_d = din("thrrow", [128, NBLK])
    out_d = nc.dram_tensor("out", [NTOK, D], F32, kind="ExternalOutput").ap()
    dbg_d = None
    if debug:
        dbg_d = nc.dram_tensor("dbg", [NTOK, D], F32, kind="ExternalOutput").ap()
        dbg2_d = nc.dram_tensor("dbg2", [128, NTT, 32], F32, kind="ExternalOutput").ap()
        dbg3_d = nc.dram_tensor("dbg3", [4, 1024], F32, kind="ExternalOutput").ap()

    ebd_d = nc.dram_tensor("ebd", [4, 128, NCLS, 2, 128], BF16, kind="Internal").ap()
    hfs_d = nc.dram_tensor("hfs", [NTOK, D], BF16, kind="Internal").ap()
    base_d = nc.dram_tensor("bases", [NTOK, D], F32, kind="Internal").ap()
    xs_d = nc.dram_tensor("xs", [NSLOT, D], BF16, kind="Internal").ap()
    ys_d = nc.dram_tensor("ys", [NSLOT, D], BF16, kind="Internal").ap()
    modrows_d = nc.dram_tensor("modrows", [nseq, 6 * D], F32, kind="Internal").ap()

    with ExitStack() as es:
        sc = Sched(nc, es)

        def chk(name):
            if stop == name:
                sc.wait_everything('sp')
                raise _Stop(nc)
        ebd_b = Buf(ebd_d, 'ebd')
        hfs_b = Buf(hfs_d, 'hfs')
        base_b = Buf(base_d, 'bases')
        xs_b = Buf(xs_d, 'xs')
        ys_b = Buf(ys_d, 'ys')
        modrows_b = Buf(modrows_d, 'modrows')
        out_b = Buf(out_d, 'out')
        dbg_b = Buf(dbg_d, 'dbg') if debug else None
        dbg2_b = Buf(dbg2_d, 'dbg2') if debug else None
        dbg3_b = Buf(dbg3_d, 'dbg3') if debug else None

        psb = [Buf(es.enter_context(nc.psum_tensor('ps%d' % i, [128, 512], F32)), 'ps%d' % i) for i in range(8)]
        pctr = [0]

        ppool = {'a': [0, 1], 'b': [2, 3, 4, 5], 'c': [6, 7]}
        pctr_p = {'a': 0, 'b': 0, 'c': 0}

        def psn(pool=None):
            if pool is None:
                b = psb[pctr[0] % 8]
                pctr[0] += 1
                return b
            lst = ppool[pool]
            b = psb[lst[pctr_p[pool] % len(lst)]]
            pctr_p[pool] += 1
            return b

        def load(dst, dst_ap, src_ap, q='sp', extra_reads=()):
            sc.dma(lambda e: e.dma_start(out=dst_ap, in_=src_ap), list(extra_reads), [dst], q=q)

        def cload(name, src, shape, dt=F32):
            b = sc.sb(name, shape, dt)
            load(b, b[:], src)
            return b

        ident = cload('ident', ident_d[:, :], [128, 128])
        identb = sc.sb('identb', [128, 128], BF16)
        sc.op('dve', lambda e: e.tensor_copy(out=identb[:], in_=ident[:]), [ident], [identb])
        lstr32 = cload('lstr32', lstrict_d[:, :], [128, 128])
        lstrb = sc.sb('lstrb', [128, 128], BF16)
        sc.op('dve', lambda e: e.tensor_copy(out=lstrb[:], in_=lstr32[:]), [lstr32], [lstrb])
        onesb = sc.sb('onesb', [128, 128], BF16)
        sc.op('pool', lambda e: e.memset(onesb[:], 1.0), [], [onesb])
        ones32 = sc.sb('ones32', [128, 256], F32)
        sc.op('pool', lambda e: e.memset(ones32[:], 1.0), [], [ones32])
        iota1 = cload('iota1', iota1_d[:, :], [128, E])
        pexp_c = cload('pexp_c', pexp_d[:, :], [128, 4])
        piota = pexp_c
        junk = sc.sb('junk', [128, 1024], F32)
        scr = {k: sc.sb('scr_' + k, [128, 2], F32) for k in ('dve', 'act', 'pool')}
        for k in scr:
            sc.op('pool', lambda e, k=k: e.memset(scr[k][:], 0.0), [], [scr[k]])
        eps_ln = sc.sb('eps_ln', [128, 1], F32)
        sc.op('pool', lambda e: e.memset(eps_ln[:], 1e-5), [], [eps_ln])
        eps_gn = sc.sb('eps_gn', [128, 1], F32)
        sc.op('pool', lambda e: e.memset(eps_gn[:], 1e-6), [], [eps_gn])

        E8 = sc.sb('E8', [128, NTT, 8], F32)
        W8 = sc.sb('W8', [128, NTT, 8], F32)
        R8 = sc.sb('R8', [128, NTT, 8], F32)
        D8i = [sc.sb('D8i%d' % g, [128, 8], I32) for g in range(NTT)]
        cnt_run = sc.sb('cnt_run', [128, E], F32)
        sc.op('pool', lambda e: e.memset(cnt_run[:], 0.0), [], [cnt_run])
        gf_all = sc.sb('gf_all', [128, nseq, D], F32)
        rb_bc = sc.sb('rb_bc', [128, E], F32)
        load(rb_bc, rb_bc[:], rb_d[0:1, :].partition_broadcast(128))

        stage = sc.sb('stage', [128, 8, 512], F32)

        def load_w_bf(dst, dst_ap3, src_cols, width):
            load(stage, stage[:, :, 0:width], src_cols.rearrange("(k p) n -> p k n", p=128))
            sc.op('dve', lambda e: e.tensor_copy(out=dst_ap3, in_=stage[:, :, 0:width]), [stage], [dst])

        chk('c0')
        if True:
            sc.push()
            ebst = sc.sb('ebst', [128, NCLS, 128], F32)
            ebbf = [sc.sb('ebbf%d' % i, [128, NCLS, 128], BF16) for i in range(2)]
            for h in range(8):
                load(ebst, ebst[:], biasT_d[h])
                eb = ebbf[h % 2]
                sc.op('act', lambda e, eb=eb: e.activation(out=eb[:], in_=ebst[:], func=AF.Exp), [ebst], [eb])
                sc.dma(lambda e, eb=eb, h=h: e.dma_start(out=ebd_d[h // 2, :, :, h % 2, :], in_=eb[:]), [eb], [ebd_b])
            sc.pop()

        chk('eb')
        sc.push()
        condT = sc.sb('condT', [128, 8, nseq], F32)
        cT_sb = sc.sb('cT_sb', [128, 8, nseq], F32)
        load(cT_sb, cT_sb[:], cT_d[:, :, :])
        sc.op('act', lambda e: e.activation(out=condT[:], in_=cT_sb[:], func=AF.Silu), [cT_sb], [condT])
        bada_sb = sc.sb('bada_sb', [1, 512], F32)
        ones_row = sc.sb('ones_row', [1, 128], F32)
        sc.op('pool', lambda e: e.memset(ones_row[:], 1.0), [], [ones_row])
        gng_bc = sc.sb('gng_bc', [128, 512], F32)
        load(gng_bc, gng_bc[:], gng_d[0:1, :].partition_broadcast(128))
        lgbc = sc.sb('lgbc', [128, 8], F32)
        load(lgbc, lgbc[:], lg_d[0:1, :].partition_broadcast(128))
        pexp = cload('pexp', pexp_d[:, :], [128, 4])

        DT = sc.sb('DT', [128, 4, 128], BF16)
        DFrow = sc.sb('DFrow', [128, 4, 128], BF16)
        DBrow = sc.sb('DBrow', [128, 4, 128], BF16)
        pdec = sc.sb('pdec', [128, 4, 4], F32)
        cdec = sc.sb('cdec', [128, 4, 2], F32)
        if True:
            sc.push()
            dpos = cload('dpos', dpos_d[:, :], [128, 128])
            dneg = cload('dneg', dneg_d[:, :], [128, 128])
            maskf = cload('maskf', maskf_d[:, :], [128, 128])
            maskb = cload('maskb', maskb_d[:, :], [128, 128])
            ifp1 = cload('ifp1', ifp1_d[:, :], [128, 128])
            ifm = cload('ifm', ifm_d[:, :], [128, 128])
            t1 = sc.sb('dt1', [128, 128], F32)
            t2 = sc.sb('dt2', [128, 128], F32)
            lg128 = sc.sb('lg128', [128, 8], F32)
            sc.op('dve', lambda e: e.tensor_scalar(out=lg128[:], in0=lgbc[:], scalar1=128.0, scalar2=None, op0=ALU.mult), [lgbc], [lg128])
            for h in range(4):
                lf = lgbc[:, h:h + 1]
                lb = lgbc[:, 4 + h:5 + h]
                sc.op('act', lambda e, lf=lf: e.activation(out=t1[:], in_=dpos[:], func=AF.Exp, scale=lf), [dpos, lgbc], [t1])
                sc.op('dve', lambda e: e.tensor_tensor(out=t1[:], in0=t1[:], in1=maskf[:], op=ALU.mult), [t1, maskf], [t1])
                sc.op('act', lambda e, lb=lb: e.activation(out=t2[:], in_=dneg[:], func=AF.Exp, scale=lb), [dneg, lgbc], [t2])
                sc.op('dve', lambda e: e.tensor_tensor(out=t2[:], in0=t2[:], in1=maskb[:], op=ALU.mult), [t2, maskb], [t2])
                sc.op('dve', lambda e, h=h: e.tensor_tensor(out=DT[:, h, :], in0=t1[:], in1=t2[:], op=ALU.add), [t1, t2], [DT])
                sc.op('act', lambda e, lf=lf, h=h: e.activation(out=DFrow[:, h, :], in_=ifp1[:], func=AF.Exp, scale=lf), [ifp1, lgbc], [DFrow])
                sc.op('act', lambda e, lb=lb, h=h: e.activation(out=DBrow[:, h, :], in_=ifm[:], func=AF.Exp, scale=lb), [ifm, lgbc], [DBrow])
                sc.op('act', lambda e, lb=lb, h=h: e.activation(out=pdec[:, h, 0:1], in_=pexp[:, 0:1], func=AF.Exp, scale=lb), [pexp, lgbc], [pdec])
                sc.op('act', lambda e, lf=lf, h=h: e.activation(out=pdec[:, h, 1:2], in_=pexp[:, 1:2], func=AF.Exp, scale=lf), [pexp, lgbc], [pdec])
                sc.op('act', lambda e, h=h: e.activation(out=cdec[:, h, 0:1], in_=lg128[:, h:h + 1], func=AF.Exp), [lg128], [cdec])
                sc.op('act', lambda e, h=h: e.activation(out=cdec[:, h, 1:2], in_=lg128[:, 4 + h:5 + h], func=AF.Exp), [lg128], [cdec])
            sc.pop()

        chk('dec')
        ynaT = sc.sb('ynaT', [128, 4, S], BF16)
        yrT = sc.sb('yrT', [128, 4, S], BF16)

        modr = [sc.sb('modr%d' % i, [nseq, 512], F32) for i in range(2)]
        for blk in range(12):
            v = blk // 2
            load(stage, stage[:], wada_d[:, blk * 512:(blk + 1) * 512].rearrange("(k p) n -> p k n", p=128))
            load(bada_sb, bada_sb[:], bada_d[0:1, blk * 512:(blk + 1) * 512])
            ps = psn()
            for k in range(8):
                sc.op('pe', lambda e, k=k, ps=ps: e.matmul(ps[0:nseq, :], lhsT=condT[:, k, :], rhs=stage[:, k, :], start=(k == 0), stop=False), [condT, stage], [ps], inc=False)
            sc.op('pe', lambda e, ps=ps: e.matmul(ps[0:nseq, :], lhsT=ones_row[0:1, 0:nseq], rhs=bada_sb[0:1, :], start=False, stop=True), [ones_row, bada_sb], [ps])
            mr = modr[blk % 2]
            if v in (1, 4):
                sc.op('dve', lambda e, ps=ps, mr=mr: e.tensor_scalar(out=mr[:], in0=ps[0:nseq, :], scalar1=1.0, scalar2=None, op0=ALU.add), [ps], [mr])
            else:
                sc.op('act', lambda e, ps=ps, mr=mr: e.activation(out=mr[:], in_=ps[0:nseq, :], func=AF.Identity), [ps], [mr])
            sc.dma(lambda e, mr=mr, blk=blk: e.dma_start(out=modrows_d[0:nseq, blk * 512:(blk + 1) * 512], in_=mr[:]), [mr], [modrows_b], q='act')

        def mod_pass(b, blks, gbc):
            for v in sorted(set(blk // 2 for blk in blks)):
                if v == 5:
                    dstb, dst = gf_all, gf_all[:, b, :]
                else:
                    dstb, dst = gbc[v], gbc[v][:]
                load(dstb, dst, modrows_d[b:b + 1, v * D:(v + 1) * D].partition_broadcast(128), extra_reads=[modrows_b])

        def ln_tile(z, outb, gb, bb, epsb, tag):
            st = lnw['st']; mv = lnw['mv']; rstd = lnw['rstd']; nb = lnw['nb']
            for hh in range(2):
                sc.op('dve', lambda e, hh=hh: e.bn_stats(out=st[:, hh, :], in_=z[:, hh * 512:(hh + 1) * 512]), [z], [st])
            sc.op('dve', lambda e: e.bn_aggr(out=mv[:], in_=st[:]), [st], [mv])
            sc.op('act', lambda e: e.activation(out=rstd[:], in_=mv[:, 1:2], func=AF.Sqrt, bias=epsb[:, 0:1], scale=1.0), [mv, epsb], [rstd])
            sc.op('dve', lambda e: e.reciprocal(out=rstd[:], in_=rstd[:]), [rstd], [rstd])
            sc.op('dve', lambda e: e.scalar_tensor_tensor(out=nb[:], in0=mv[:, 0:1], scalar=-1.0, in1=rstd[:], op0=ALU.mult, op1=ALU.mult), [mv, rstd], [nb])
            sc.op('act', lambda e: e.activation(out=z[:], in_=z[:], func=AF.Identity, bias=nb[:, 0:1], scale=rstd[:, 0:1]), [z, nb, rstd], [z])
            sc.op('dve', lambda e: e.tensor_tensor(out=z[:], in0=z[:], in1=gb[:], op=ALU.mult), [z, gb], [z])
            sc.op('dve', lambda e: e.tensor_tensor(out=outb[:], in0=z[:], in1=bb[:], op=ALU.add), [z, bb], [outb])

        SD = nc.vector.BN_STATS_DIM
        AD = nc.vector.BN_AGGR_DIM
        lnw = {'st': sc.sb('ln_st', [128, 2, SD], F32), 'mv': sc.sb('ln_mv', [128, AD], F32),
               'rstd': sc.sb('ln_rstd', [128, 1], F32), 'nb': sc.sb('ln_nb', [128, 1], F32)}

        for b in range(nseq):
            sc.push()
            hT = sc.sb('hT', [128, 8, S], BF16)
            sc.push()
            gbc = {v: sc.sb('gbc%d' % v, [128, D], F32) for v in (0, 1)}
            xt = [sc.sb('xt%d' % i, [128, D], F32) for i in range(2)]
            hb = [sc.sb('hb%d' % i, [128, D], BF16) for i in range(2)]
            mod_pass(b, range(0, 4), gbc)
            chk('mod')
            for t in range(NT):
                r0 = (b * NT + t) * 128
                xb = xt[t % 2]
                hbb = hb[t % 2]
                load(xb, xb[:], x_d[r0:r0 + 128, :])
                sc.op('dve', lambda e, xb=xb: e.tensor_tensor(out=junk[:], in0=xb[:], in1=gbc[1][:], op=ALU.mult), [xb, gbc[1]], [junk])
                sc.op('dve', lambda e, hbb=hbb: e.tensor_tensor(out=hbb[:], in0=junk[:], in1=gbc[0][:], op=ALU.add), [junk, gbc[0]], [hbb])
                ps = psn()
                pv = ps[:, :].bitcast(BF16)
                for k in range(8):
                    sc.op('pe', lambda e, k=k, pv=pv, hbb=hbb: e.transpose(pv[:, k * 128:(k + 1) * 128], hbb[:, k * 128:(k + 1) * 128], identb[:]),
                          [hbb, identb], [ps], inc=(k == 7))
                sc.op('act', lambda e, pv=pv, t=t: e.activation(out=hT[:, :, t * 128:(t + 1) * 128], in_=pv.rearrange("p (k n) -> p k n", k=8), func=AF.Identity),
                      [ps], [hT])

            chk('a1')
            sc.pop()
            if True:
                sc.push()
                wv = sc.sb('wv', [128, 8, 512], BF16)
                vaug = sc.sb('vaug', [128, NT, 8, 66], BF16)
                sc.op('pool', lambda e: e.memset(vaug[:], 1.0), [], [vaug])
                load_w_bf(wv, wv[:], win_d[:, 1024:1536], 512)
                for t in range(NT):
                    ps = psn()
                    for k in range(8):
                        sc.op('pe', lambda e, k=k, ps=ps, t=t: e.matmul(ps[:, :], lhsT=hT[:, k, t * 128:(t + 1) * 128], rhs=wv[:, k, :], start=(k == 0), stop=(k == 7)),
                              [hT, wv], [ps], inc=(k == 7))
                    sc.op('act', lambda e, ps=ps, t=t: e.activation(out=vaug[:, t, :, 0:64], in_=ps[:, :].rearrange("p (h d) -> p h d", h=8), func=AF.Identity), [ps], [vaug])
                chk('na_v')
                wq = sc.sb('wq', [128, 8, 128], BF16)
                wk = sc.sb('wk', [128, 8, 128], BF16)
                qT = sc.sb('qT', [128, 2, S], BF16)
                sc.op('pool', lambda e: e.memset(qT[:], 0.0), [], [qT])
                kT = sc.sb('kT', [128, S], BF16)
                EBh = sc.sb('EBh', [128, NCLS, 2, 128], BF16)
                pes = [sc.sb('pes%d' % i, [128, 256], BF16) for i in range(4)]
                pts = [sc.sb('pts%d' % i, [128, 256], BF16) for i in range(4)]
                rden = sc.sb('rden', [128, 2], F32)
                ynat = [sc.sb('ynat%d' % i, [128, 128], BF16) for i in range(2)]
                pctr2 = 0
                for hp in range(4):
                    load_w_bf(wq, wq[:], win_d[:, hp * 128:(hp + 1) * 128], 128)
                    load_w_bf(wk, wk[:], win_d[:, 512 + hp * 128:512 + (hp + 1) * 128], 128)
                    load(EBh, EBh[:], ebd_d[hp], extra_reads=[ebd_b])
                    for nchunk in range(4):
                        for (wsrc, dstT, scl) in ((wq, qT, 0.125), (wk, kT, 1.0)):
                            ps = psn()
                            for k in range(8):
                                sc.op('pe', lambda e, k=k, ps=ps, wsrc=wsrc, nchunk=nchunk: e.matmul(ps[:, :], lhsT=wsrc[:, k, :], rhs=hT[:, k, nchunk * 512:(nchunk + 1) * 512], start=(k == 0), stop=(k == 7)),
                                      [wsrc, hT], [ps], inc=(k == 7))
                            cs_ = slice(nchunk * 512, (nchunk + 1) * 512)
                            if dstT is qT:
                                sc.op('act', lambda e, ps=ps, cs_=cs_: e.activation(out=qT[0:64, 0, cs_], in_=ps[0:64, :], func=AF.Identity, scale=0.125), [ps], [qT])
                                sc.op('act', lambda e, ps=ps, cs_=cs_: e.activation(out=qT[64:128, 1, cs_], in_=ps[64:128, :], func=AF.Identity, scale=0.125), [ps], [qT])
                            else:
                                sc.op('act', lambda e, ps=ps, cs_=cs_: e.activation(out=kT[:, cs_], in_=ps[:, :], func=AF.Identity), [ps], [kT])
                    chk('na_qk')
                    iters = []
                    for j in range(NT):
                        kts = [kt for kt in range(NT) if (j, kt) in CLS_OF]
                        for ki, kt in enumerate(kts):
                            iters.append((j, kt, CLS_OF[(j, kt)], ki == 0, ki == len(kts) - 1))
                    ps_s_l = {}

                    def emit_S(ii):
                        j, kt, cls, first, last = iters[ii]
                        ps_s = psn('b')
                        ps_s_l[ii] = ps_s
                        for hh in range(2):
                            sc.op('pe', lambda e, hh=hh, ps_s=ps_s, kt=kt, j=j: e.matmul(ps_s[:, hh * 128:(hh + 1) * 128], lhsT=kT[:, kt * 128:(kt + 1) * 128],
                                                                                 rhs=qT[:, hh, j * 128:(j + 1) * 128], start=True, stop=True),
                                  [kT, qT], [ps_s], inc=(hh == 1))
                    for ii in range(min(2, len(iters))):
                        emit_S(ii)
                    ps_pv = None
                    for ii, (j, kt, cls, first, last) in enumerate(iters):
                        if first:
                            ps_pv = psn('a')
                        ps_s = ps_s_l.pop(ii)
                        pe_sb = pes[ii % 4]
                        pt_sb = pts[ii % 4]
                        sc.op('act', lambda e, pe_sb=pe_sb, ps_s=ps_s: e.activation(out=pe_sb[:], in_=ps_s[:, 0:256], func=AF.Exp), [ps_s], [pe_sb])
                        sc.op('dve', lambda e, pe_sb=pe_sb, pt_sb=pt_sb, cls=cls: e.tensor_tensor(out=pt_sb[:], in0=pe_sb[:], in1=EBh[:, cls, :, :].rearrange("p a b -> p (a b)"), op=ALU.mult),
                              [pe_sb, EBh], [pt_sb])
                        if ii + 2 < len(iters):
                            emit_S(ii + 2)
                        for hh in range(2):
                            h = hp * 2 + hh
                            sc.op('pe', lambda e, hh=hh, h=h, pt_sb=pt_sb, kt=kt, first=first, last=last, ps_pv=ps_pv: e.matmul(ps_pv[:, hh * 128:hh * 128 + 66], lhsT=pt_sb[:, hh * 128:(hh + 1) * 128], rhs=vaug[:, kt, h, 0:66],
                                                                                                                     start=first, stop=last),
                                  [pt_sb, vaug], [ps_pv], inc=(hh == 1))
                        if not last:
                            continue
                        yt_ = ynat[j % 2]
                        sc.op('dve', lambda e, ps_pv=ps_pv: e.reciprocal(out=rden[:], in_=ps_pv[:, 0:256].rearrange("p (a b) -> p a b", a=2)[:, :, 64]), [ps_pv], [rden])
                        for hh in range(2):
                            sc.op('dve', lambda e, hh=hh, ps_pv=ps_pv, yt_=yt_: e.tensor_scalar(out=yt_[:, hh * 64:(hh + 1) * 64], in0=ps_pv[:, hh * 128:hh * 128 + 64], scalar1=rden[:, hh:hh + 1], scalar2=None, op0=ALU.mult),
                                  [ps_pv, rden], [yt_])
                        ps_t = psn('c')
                        ptv = ps_t[:, :].bitcast(BF16)
                        sc.op('pe', lambda e, ptv=ptv, yt_=yt_: e.transpose(ptv[:, 0:128], yt_[:], identb[:]), [yt_, identb], [ps_t])
                        sc.op('act', lambda e, ptv=ptv, hp=hp, j=j: e.activation(out=ynaT[:, hp, j * 128:(j + 1) * 128], in_=ptv[:, 0:128], func=AF.Identity), [ps_t], [ynaT])
                sc.pop()

            chk('na')
            if True:
                sc.push()
                wrh = sc.sb('wrh', [128, 8, 512], BF16)
                cos_sb = cload('cos_sb', cos_d[:, :, :], [128, NT, 64])
                sin_sb = cload('sin_sb', sin_d[:, :, :], [128, NT, 64])
                nsin_sb = cload('nsin_sb', nsin_d[:, :, :], [128, NT, 64])
                qTr = sc.sb('qTr', [128, S], BF16)
                kTr = sc.sb('kTr', [128, S], BF16)
                ktok = sc.sb('ktok', [128, NT, 128], BF16)
                vtok = sc.sb('vtok', [128, NT, 128], BF16)
                sg = sc.sb('sg', [128, NT, 128], BF16)
                Rb_bf = sc.sb('Rb_bf', [128, NT, 128], BF16)
                Rst = sc.sb('Rst', [128, 128], F32)
                Rf_bf = [sc.sb('Rf_bf%d' % i, [128, 128], BF16) for i in range(2)]
                ra = sc.sb('ra', [128, 256], F32)
                rbq = sc.sb('rbq', [128, 2, 128], F32)
                qkb = [sc.sb('qkb%d' % i, [128, 2, 128], BF16) for i in range(2)]
                sgt = sc.sb('sgt', [128, 128], F32)
                kd = [sc.sb('kd%d' % i, [128, 128], BF16) for i in range(2)]
                AT = [sc.sb('AT%d' % i, [128, 128], BF16) for i in range(2)]
                qfT = [sc.sb('qfT%d' % i, [128, 128], BF16) for i in range(2)]
                qbT = [sc.sb('qbT%d' % i, [128, 128], BF16) for i in range(2)]
                gst = sc.sb('gst', [128, SD], F32)
                gmv = sc.sb('gmv', [128, AD], F32)
                grs = sc.sb('grs', [128, 1], F32)
                gnb = sc.sb('gnb', [128, 1], F32)
                yn = sc.sb('yn', [128, 128], F32)
                yrt = [sc.sb('yrt%d' % i, [128, 128], BF16) for i in range(2)]
                for h in range(4):
                    for gi, c0 in enumerate((1536, 2048, 2560, 3072)):
                        load(stage, stage[:, :, gi * 128:(gi + 1) * 128], win_d[:, c0 + h * 128:c0 + (h + 1) * 128].rearrange("(k p) n -> p k n", p=128))
                    sc.op('dve', lambda e: e.tensor_copy(out=wrh[:], in_=stage[:]), [stage], [wrh])
                    pend_T = []

                    def emit_qkT(n, qk):
                        ps_t = psn('c')
                        ptv = ps_t[:, :].bitcast(BF16)
                        sc.op('pe', lambda e: e.transpose(ptv[:, 0:128], qk[:, 0, :], identb[:]), [qk, identb], [ps_t], inc=False)
                        sc.op('pe', lambda e: e.transpose(ptv[:, 128:256], ktok[:, n, :], identb[:]), [ktok, identb], [ps_t])
                        sc.op('act', lambda e: e.activation(out=qTr[:, n * 128:(n + 1) * 128], in_=ptv[:, 0:128], func=AF.Identity), [ps_t], [qTr])
                        sc.op('act', lambda e: e.activation(out=kTr[:, n * 128:(n + 1) * 128], in_=ptv[:, 128:256], func=AF.Identity), [ps_t], [kTr])
                    for n in range(NT):
                        ps = psn()
                        for k in range(8):
                            sc.op('pe', lambda e, k=k, ps=ps, n=n: e.matmul(ps[:, :], lhsT=hT[:, k, n * 128:(n + 1) * 128], rhs=wrh[:, k, :], start=(k == 0), stop=(k == 7)),
                                  [hT, wrh], [ps], inc=(k == 7))
                        cosb = cos_sb[:, n, :].unsqueeze(1).to_broadcast([128, 4, 64])
                        sc.op('dve', lambda e, ps=ps, cosb=cosb: e.tensor_tensor(out=ra[:].rearrange("p (a d) -> p a d", a=4), in0=ps[:, 0:256].rearrange("p (a d) -> p a d", a=4), in1=cosb, op=ALU.mult),
                              [ps, cos_sb], [ra])
                        p3 = ps[:, 0:256].rearrange("p (a d) -> p a d", a=2)
                        sc.op('dve', lambda e, p3=p3, n=n: e.tensor_tensor(out=rbq[:, :, 0:64], in0=p3[:, :, 64:128], in1=nsin_sb[:, n, :].unsqueeze(1).to_broadcast([128, 2, 64]), op=ALU.mult),
                              [ps, nsin_sb], [rbq])
                        sc.op('dve', lambda e, p3=p3, n=n: e.tensor_tensor(out=rbq[:, :, 64:128], in0=p3[:, :, 0:64], in1=sin_sb[:, n, :].unsqueeze(1).to_broadcast([128, 2, 64]), op=ALU.mult),
                              [ps, sin_sb], [rbq])
                        qk = qkb[n % 2]
                        sc.op('dve', lambda e, qk=qk: e.tensor_tensor(out=qk[:].rearrange("p a d -> p (a d)"), in0=ra[:], in1=rbq[:].rearrange("p a d -> p (a d)"), op=ALU.add), [ra, rbq], [qk])
                        sc.op('act', lambda e, qk=qk, n=n: e.activation(out=ktok[:, n, :], in_=qk[:, 1, :], func=AF.Identity, scale=float(128.0 ** -0.5)), [qk], [ktok])
                        sc.op('act', lambda e, ps=ps, n=n: e.activation(out=vtok[:, n, :], in_=ps[:, 256:384], func=AF.Identity), [ps], [vtok])
                        sc.op('act', lambda e, ps=ps: e.activation(out=sgt[:], in_=ps[:, 384:512], func=AF.Silu), [ps], [sgt])
                        sc.op('dve', lambda e, n=n, h=h: e.tensor_tensor(out=sg[:, n, :], in0=sgt[:], in1=gng_bc[:, h * 128:(h + 1) * 128], op=ALU.mult), [sgt, gng_bc], [sg])
                        pend_T.append((n, qk))
                        if len(pend_T) > 1:
                            emit_qkT(*pend_T.pop(0))
                    emit_qkT(*pend_T.pop(0))
                    sc.op('pool', lambda e: e.memset(Rst[:], 0.0), [], [Rst])
                    sc.op('pool', lambda e: e.memset(Rb_bf[:, NT - 1, :], 0.0), [], [Rb_bf])
                    for n in range(NT - 1, 0, -1):
                        kdn = kd[n % 2]
                        sc.op('act', lambda e, kdn=kdn, n=n, h=h: e.activation(out=kdn[:], in_=ktok[:, n, :], func=AF.Identity, scale=pdec[:, h, 0:1]), [ktok, pdec], [kdn])
                        ps = psn()
                        sc.op('pe', lambda e, ps=ps, kdn=kdn, n=n: e.matmul(ps[:, 0:128], lhsT=kdn[:], rhs=vtok[:, n, :], start=True, stop=True), [kdn, vtok], [ps])
                        sc.op('dve', lambda e, ps=ps, h=h: e.scalar_tensor_tensor(out=Rst[:], in0=Rst[:], scalar=cdec[:, h, 1:2], in1=ps[:, 0:128], op0=ALU.mult, op1=ALU.add), [Rst, cdec, ps], [Rst])
                        sc.op('act', lambda e, n=n: e.activation(out=Rb_bf[:, n - 1, :], in_=Rst[:], func=AF.Identity), [Rst], [Rb_bf])
                    sc.op('pool', lambda e: e.memset(Rst[:], 0.0), [], [Rst])
                    sc.op('pool', lambda e: e.memset(Rf_bf[0][:], 0.0), [], [Rf_bf[0]])
                    ps_s_d = {}

                    def emit_Sr(n):
                        ps_s = psn('b')
                        ps_s_d[n] = ps_s
                        sc.op('pe', lambda e: e.matmul(ps_s[:, 0:128], lhsT=kTr[:, n * 128:(n + 1) * 128], rhs=qTr[:, n * 128:(n + 1) * 128], start=True, stop=True), [kTr, qTr], [ps_s])

                    def emit_T(n):
                        yr_ = yrt[n % 2]
                        ps_t = psn('c')
                        ptv = ps_t[:, :].bitcast(BF16)
                        sc.op('pe', lambda e: e.transpose(ptv[:, 0:128], yr_[:], identb[:]), [yr_, identb], [ps_t])
                        sc.op('act', lambda e: e.activation(out=yrT[:, h, n * 128:(n + 1) * 128], in_=ptv[:, 0:128], func=AF.Identity), [ps_t], [yrT])

                    emit_Sr(0)
                    for n in range(NT):
                        rf = Rf_bf[n % 2]
                        rfn = Rf_bf[(n + 1) % 2]
                        ps_s = ps_s_d.pop(n)
                        at = AT[n % 2]
                        qf = qfT[n % 2]
                        qb = qbT[n % 2]
                        sc.op('dve', lambda e, at=at, ps_s=ps_s, h=h: e.tensor_tensor(out=at[:], in0=ps_s[:, 0:128], in1=DT[:, h, :], op=ALU.mult), [ps_s, DT], [at])
                        sc.op('dve', lambda e, qf=qf, n=n, h=h: e.tensor_tensor(out=qf[:], in0=qTr[:, n * 128:(n + 1) * 128], in1=DFrow[:, h, :], op=ALU.mult), [qTr, DFrow], [qf])
                        sc.op('dve', lambda e, qb=qb, n=n, h=h: e.tensor_tensor(out=qb[:], in0=qTr[:, n * 128:(n + 1) * 128], in1=DBrow[:, h, :], op=ALU.mult), [qTr, DBrow], [qb])
                        ps_y = psn('a')
                        sc.op('pe', lambda e, ps_y=ps_y, at=at, n=n: e.matmul(ps_y[:, 0:128], lhsT=at[:], rhs=vtok[:, n, :], start=True, stop=False), [at, vtok], [ps_y], inc=False)
                        sc.op('pe', lambda e, ps_y=ps_y, qf=qf, rf=rf: e.matmul(ps_y[:, 0:128], lhsT=qf[:], rhs=rf[:], start=False, stop=False), [qf, rf], [ps_y], inc=False)
                        sc.op('pe', lambda e, ps_y=ps_y, qb=qb, n=n: e.matmul(ps_y[:, 0:128], lhsT=qb[:], rhs=Rb_bf[:, n, :], start=False, stop=True), [qb, Rb_bf], [ps_y])
                        if n > 0:
                            emit_T(n - 1)
                        if n < NT - 1:
                            kdn = kd[n % 2]
                            sc.op('act', lambda e, kdn=kdn, n=n, h=h: e.activation(out=kdn[:], in_=ktok[:, n, :], func=AF.Identity, scale=pdec[:, h, 1:2]), [ktok, pdec], [kdn])
                            ps = psn('b')
                            sc.op('pe', lambda e, ps=ps, kdn=kdn, n=n: e.matmul(ps[:, 0:128], lhsT=kdn[:], rhs=vtok[:, n, :], start=True, stop=True), [kdn, vtok], [ps])
                            sc.op('dve', lambda e, ps=ps, h=h: e.scalar_tensor_tensor(out=Rst[:], in0=Rst[:], scalar=cdec[:, h, 0:1], in1=ps[:, 0:128], op0=ALU.mult, op1=ALU.add), [Rst, cdec, ps], [Rst])
                            sc.op('act', lambda e, rfn=rfn: e.activation(out=rfn[:], in_=Rst[:], func=AF.Identity), [Rst], [rfn])
                            emit_Sr(n + 1)
                        sc.op('dve', lambda e, ps_y=ps_y: e.bn_stats(out=gst[:], in_=ps_y[:, 0:128]), [ps_y], [gst])
                        sc.op('dve', lambda e: e.bn_aggr(out=gmv[:], in_=gst[:]), [gst], [gmv])
                        sc.op('act', lambda e: e.activation(out=grs[:], in_=gmv[:, 1:2], func=AF.Sqrt, bias=eps_gn[:, 0:1], scale=1.0), [gmv, eps_gn], [grs])
                        sc.op('dve', lambda e: e.reciprocal(out=grs[:], in_=grs[:]), [grs], [grs])
                        sc.op('dve', lambda e: e.scalar_tensor_tensor(out=gnb[:], in0=gmv[:, 0:1], scalar=-1.0, in1=grs[:], op0=ALU.mult, op1=ALU.mult), [gmv, grs], [gnb])
                        sc.op('act', lambda e, ps_y=ps_y: e.activation(out=yn[:], in_=ps_y[:, 0:128], func=AF.Identity, bias=gnb[:, 0:1], scale=grs[:, 0:1]), [ps_y, gnb, grs], [yn])
                        yr_ = yrt[n % 2]
                        sc.op('dve', lambda e, yr_=yr_, n=n: e.tensor_tensor(out=yr_[:], in0=yn[:], in1=sg[:, n, :], op=ALU.mult), [yn, sg], [yr_])
                    emit_T(NT - 1)
                sc.pop()

            chk('ret')
            sc.pop()
            if True:
                sc.push()
                gbc = {v: sc.sb('gbc%d' % v, [128, D], F32) for v in (2, 3, 4)}
                xt = [sc.sb('xt%d' % i, [128, D], F32) for i in range(2)]
                lnbc = [sc.sb('ln1bc%d' % v, [128, D], F32) for v in range(2)]
                for v in range(2):
                    load(lnbc[v], lnbc[v][:], ln_d[v:v + 1, :].partition_broadcast(128))
                mod_pass(b, range(4, 12), gbc)
                wo = sc.sb('wo', [128, 8, D], BF16)
                for half in range(2):
                    load_w_bf(wo, wo[:, :, half * 512:(half + 1) * 512], wout_d[:, half * 512:(half + 1) * 512], 512)
                wsgu = sc.sb('wsgu', [128, 8, 512], BF16)
                load_w_bf(wsgu, wsgu[:, :, 0:256], wsg_d[:, :], 256)
                load_w_bf(wsgu, wsgu[:, :, 256:512], wsu_d[:, :], 256)
                wsd = sc.sb('wsd', [128, 2, D], BF16)
                load(stage, stage[:, 0:2, :].rearrange("p a (b n) -> p a b n", b=1)[:, :, 0, :], wsd_d[:, 0:512].rearrange("(k p) n -> p k n", p=128))
                sc.op('dve', lambda e: e.tensor_copy(out=wsd[:, :, 0:512], in_=stage[:, 0:2, :]), [stage], [wsd])
                load(stage, stage[:, 0:2, :], wsd_d[:, 512:1024].rearrange("(k p) n -> p k n", p=128))
                sc.op('dve', lambda e: e.tensor_copy(out=wsd[:, :, 512:1024], in_=stage[:, 0:2, :]), [stage], [wsd])
                wr32 = sc.sb('wr32', [128, 8, E], F32)
                load(wr32, wr32[:], wr_d[:, :].rearrange("(k p) n -> p k n", p=128))
                z = sc.sb('z', [128, D], F32)
                x1l = [sc.sb('x1_%d' % i, [128, D], F32) for i in range(2)]
                hf32 = sc.sb('hf32', [128, D], F32)
                hfb = [sc.sb('hfb%d' % i, [128, D], BF16) for i in range(1)]
                hfT32 = sc.sb('hfT32', [128, 8, 128], F32)
                hfTbl = [sc.sb('hfTb%d' % i, [128, 8, 128], BF16) for i in range(2)]
                basef = [sc.sb('basef%d' % i, [128, D], F32) for i in range(1)]
                scsl = [sc.sb('scs%d' % i, [128, E], F32) for i in range(2)]
                sel = sc.sb('sel', [128, E], F32)
                m8 = sc.sb('m8', [128, 8, 8], F32)
                gs = sc.sb('gs', [128, 8], F32)
                g8 = sc.sb('g8', [128, 8], F32)
                gm = sc.sb('gm', [128, 8], F32)
                pen = sc.sb('pen', [128, 8], F32)
                msk = sc.sb('msk', [128, E], F32)
                t8 = sc.sb('t8', [128, 8], F32)
                selm = sc.sb('selm', [128, E], F32)
                selb = sc.sb('selb', [128, E], BF16)
                G = sc.sb('G', [128, E], F32)
                ssum = sc.sb('ssum', [128, 1], F32)
                rank = sc.sb('rank', [128, E], F32)
                keye = sc.sb('keye', [128, E], F32)
                abf = sc.sb('abf', [128, 256], BF16)
                sgu = sc.sb('sgu', [128, 256], F32)
                aT = sc.sb('aT', [128, 2, 128], BF16)
                def stage_X(t):
                    gt = b * NT + t
                    r0 = gt * 128
                    cols = slice(t * 128, (t + 1) * 128)
                    x1 = x1l[t % 2]
                    hfTb = hfTbl[t % 2]
                    scs = scsl[t % 2]
                    ps_m = [psn(), psn()]
                    for half in range(2):
                        for k in range(8):
                            src = ynaT if k < 4 else yrT
                            sc.op('pe', lambda e, k=k, half=half, src=src, cols=cols, ps_m=ps_m: e.matmul(ps_m[half][:, :], lhsT=src[:, k % 4, cols], rhs=wo[:, k, half * 512:(half + 1) * 512], start=(k == 0), stop=(k == 7)),
                                  [src, wo], [ps_m[half]], inc=(k == 7))
                    xb = xt[t % 2]
                    load(xb, xb[:], x_d[r0:r0 + 128, :])
                    for half in range(2):
                        hs = slice(half * 512, (half + 1) * 512)
                        sc.op('dve', lambda e, half=half, hs=hs, ps_m=ps_m: e.tensor_tensor(out=z[:, hs], in0=ps_m[half][:, :], in1=gbc[2][:, hs], op=ALU.mult), [ps_m[half], gbc[2]], [z])
                    sc.op('dve', lambda e, xb=xb: e.scalar_tensor_tensor(out=z[:], in0=xb[:], scalar=ALPHA, in1=z[:], op0=ALU.mult, op1=ALU.add), [xb, z], [z])
                    ln_tile(z, x1, lnbc[0], lnbc[1], eps_ln, 'ln1')
                    if debug and b == 0:
                        sc.dma(lambda e, r0=r0: e.dma_start(out=dbg_d[r0:r0 + 128, :], in_=x1[:]), [x1], [dbg_b])
                    sc.op('pool', lambda e: e.tensor_tensor(out=hf32[:], in0=x1[:], in1=gbc[4][:], op=ALU.mult), [x1, gbc[4]], [hf32])
                    sc.op('pool', lambda e: e.tensor_tensor(out=hf32[:], in0=hf32[:], in1=gbc[3][:], op=ALU.add), [hf32, gbc[3]], [hf32])
                    hfbb = hfb[0]
                    sc.op('act', lambda e, hfbb=hfbb: e.activation(out=hfbb[:], in_=hf32[:], func=AF.Identity), [hf32], [hfbb])
                    sc.dma(lambda e, r0=r0, hfbb=hfbb: e.dma_start(out=hfs_d[r0:r0 + 128, :], in_=hfbb[:]), [hfbb], [hfs_b], q='act')
                    for half in range(2):
                        ps = psn()
                        for kk in range(4):
                            k = half * 4 + kk
                            sc.op('pe', lambda e, k=k, kk=kk, ps=ps: e.transpose(ps[:, kk * 128:(kk + 1) * 128], hf32[:, k * 128:(k + 1) * 128], ident[:]), [hf32, ident], [ps], inc=(kk == 3))
                        sc.op('act', lambda e, ps=ps, half=half: e.activation(out=hfT32[:, half * 4:(half + 1) * 4, :], in_=ps[:, :].rearrange("p (k n) -> p k n", k=4), func=AF.Identity), [ps], [hfT32])
                    sc.op('act', lambda e: e.activation(out=hfTb[:], in_=hfT32[:], func=AF.Identity), [hfT32], [hfTb])
                    ps_l = psn()
                    for k in range(8):
                        sc.op('pe', lambda e, k=k, ps_l=ps_l: e.matmul(ps_l[:, 0:E], lhsT=hfT32[:, k, :], rhs=wr32[:, k, :], start=(k == 0), stop=(k == 7)), [hfT32, wr32], [ps_l], inc=(k == 7))
                    sc.op('act', lambda e, ps_l=ps_l: e.activation(out=scs[:], in_=ps_l[:, 0:E], func=AF.Sigmoid), [ps_l], [scs])
                def stage_Y(t):
                    gt = b * NT + t
                    r0 = gt * 128
                    cols = slice(t * 128, (t + 1) * 128)
                    x1 = x1l[t % 2]
                    hfTb = hfTbl[t % 2]
                    scs = scsl[t % 2]
                    sc.op('dve', lambda e: e.tensor_tensor(out=sel[:], in0=scs[:], in1=rb_bc[:], op=ALU.add), [scs, rb_bc], [sel])
                    for g in range(8):
                        sc.op('dve', lambda e, g=g: e.max(out=m8[:, g, :], in_=sel[:, g * 32:(g + 1) * 32]), [sel], [m8])
                    sc.op('dve', lambda e: e.tensor_tensor(out=gs[:], in0=m8[:, :, 0], in1=m8[:, :, 1], op=ALU.add), [m8], [gs])
                    sc.op('dve', lambda e: e.max(out=g8[:], in_=gs[:]), [gs], [g8])
                    sc.op('dve', lambda e: e.tensor_scalar(out=gm[:], in0=gs[:], scalar1=g8[:, 3:4], scalar2=None, op0=ALU.is_ge), [gs, g8], [gm])
                    sc.op('dve', lambda e: e.tensor_scalar(out=pen[:], in0=gm[:], scalar1=-1.0, scalar2=1e9, op0=ALU.add, op1=ALU.mult), [gm], [pen])
                    sc.op('dve', lambda e: e.tensor_tensor(out=msk[:].rearrange("p (g i) -> p g i", g=8), in0=sel[:].rearrange("p (g i) -> p g i", g=8), in1=gm[:].unsqueeze(2).to_broadcast([128, 8, 32]), op=ALU.mult), [sel, gm], [msk])
                    sc.op('dve', lambda e: e.tensor_tensor(out=msk[:].rearrange("p (g i) -> p g i", g=8), in0=msk[:].rearrange("p (g i) -> p g i", g=8), in1=pen[:].unsqueeze(2).to_broadcast([128, 8, 32]), op=ALU.add), [msk, pen], [msk])
                    sc.op('dve', lambda e: e.max(out=t8[:], in_=msk[:]), [msk], [t8])
                    sc.op('dve', lambda e: e.tensor_scalar(out=selm[:], in0=msk[:], scalar1=t8[:, 7:8], scalar2=None, op0=ALU.is_ge), [msk, t8], [selm])
                    sc.op('act', lambda e: e.activation(out=selb[:], in_=selm[:], func=AF.Identity), [selm], [selb])
                    sc.op('dve', lambda e: e.tensor_tensor(out=G[:], in0=scs[:], in1=selm[:], op=ALU.mult), [scs, selm], [G])
                    sc.op('dve', lambda e: e.reduce_sum(out=ssum[:], in_=G[:], axis=AX.X), [G], [ssum])
                    sc.op('dve', lambda e: e.reciprocal(out=ssum[:], in_=ssum[:]), [ssum], [ssum])
                    sc.op('dve', lambda e: e.tensor_scalar(out=G[:], in0=G[:], scalar1=ssum[:, 0:1], scalar2=2.5, op0=ALU.mult, op1=ALU.mult), [G, ssum], [G])
                    ps_r = psn()
                    sc.op('pe', lambda e, ps_r=ps_r: e.matmul(ps_r[:, 0:E], lhsT=lstrb[:], rhs=selb[:], start=True, stop=True), [lstrb, selb], [ps_r])
                    sc.op('dve', lambda e, ps_r=ps_r: e.tensor_tensor(out=rank[:], in0=ps_r[:, 0:E], in1=cnt_run[:], op=ALU.add), [ps_r, cnt_run], [rank])
                    ps_c = psn()
                    sc.op('pe', lambda e, ps_c=ps_c: e.matmul(ps_c[:, 0:E], lhsT=onesb[:], rhs=selb[:], start=True, stop=True), [onesb, selb], [ps_c])
                    sc.op('dve', lambda e, ps_c=ps_c: e.tensor_tensor(out=cnt_run[:], in0=ps_c[:, 0:E], in1=cnt_run[:], op=ALU.add), [ps_c, cnt_run], [cnt_run])
                    sc.op('dve', lambda e: e.tensor_tensor(out=keye[:], in0=selm[:], in1=iota1[:], op=ALU.mult), [selm, iota1], [keye])
                    sc.op('dve', lambda e, gt=gt: e.max(out=E8[:, gt, :], in_=keye[:]), [keye], [E8])
                    sc.op('dve', lambda e: e.scalar_tensor_tensor(out=rank[:], in0=keye[:], scalar=8192.0, in1=rank[:], op0=ALU.mult, op1=ALU.add), [keye, rank], [rank])
                    sc.op('dve', lambda e: e.tensor_tensor(out=rank[:], in0=rank[:], in1=selm[:], op=ALU.mult), [rank, selm], [rank])
                    sc.op('dve', lambda e: e.max(out=t8[:], in_=rank[:]), [rank], [t8])
                    sc.op('dve', lambda e, gt=gt: e.scalar_tensor_tensor(out=R8[:, gt, :], in0=E8[:, gt, :], scalar=-8192.0, in1=t8[:], op0=ALU.mult, op1=ALU.add), [E8, t8], [R8])
                    sc.op('dve', lambda e: e.scalar_tensor_tensor(out=keye[:], in0=keye[:], scalar=4.0, in1=G[:], op0=ALU.mult, op1=ALU.add), [keye, G], [keye])
                    sc.op('dve', lambda e: e.max(out=g8[:], in_=keye[:]), [keye], [g8])
                    sc.op('dve', lambda e, gt=gt: e.scalar_tensor_tensor(out=W8[:, gt, :], in0=E8[:, gt, :], scalar=-4.0, in1=g8[:], op0=ALU.mult, op1=ALU.add), [E8, g8], [W8])
                    ps_g = psn()
                    for k in range(8):
                        sc.op('pe', lambda e, k=k, ps_g=ps_g: e.matmul(ps_g[:, :], lhsT=hfTb[:, k, :], rhs=wsgu[:, k, :], start=(k == 0), stop=(k == 7)), [hfTb, wsgu], [ps_g], inc=(k == 7))
                    sc.op('act', lambda e, ps_g=ps_g: e.activation(out=sgu[:], in_=ps_g[:, 0:256], func=AF.Silu), [ps_g], [sgu])
                    sc.op('dve', lambda e, ps_g=ps_g: e.tensor_tensor(out=abf[:], in0=sgu[:], in1=ps_g[:, 256:512], op=ALU.mult), [sgu, ps_g], [abf])
                    ps_t = psn()
                    ptv = ps_t[:, :].bitcast(BF16)
                    for kk in range(2):
                        sc.op('pe', lambda e, kk=kk, ptv=ptv: e.transpose(ptv[:, kk * 128:(kk + 1) * 128], abf[:, kk * 128:(kk + 1) * 128], identb[:]), [abf, identb], [ps_t], inc=(kk == 1))
                    sc.op('act', lambda e, ptv=ptv: e.activation(out=aT[:].rearrange("p a n -> p (a n)"), in_=ptv[:, 0:256], func=AF.Identity), [ps_t], [aT])
                    bf_ = basef[0]
                    for half in range(2):
                        hs = slice(half * 512, (half + 1) * 512)
                        ps = psn()
                        for kk in range(2):
                            sc.op('pe', lambda e, kk=kk, ps=ps, hs=hs: e.matmul(ps[:, :], lhsT=aT[:, kk, :], rhs=wsd[:, kk, hs], start=(kk == 0), stop=(kk == 1)), [aT, wsd], [ps], inc=(kk == 1))
                        sc.op('dve', lambda e, ps=ps, hs=hs, bf_=bf_, b=b: e.tensor_tensor(out=bf_[:, hs], in0=ps[:, :], in1=gf_all[:, b, hs], op=ALU.mult), [ps, gf_all], [bf_])
                    sc.op('dve', lambda e, bf_=bf_: e.scalar_tensor_tensor(out=bf_[:], in0=x1[:], scalar=ALPHA, in1=bf_[:], op0=ALU.mult, op1=ALU.add), [x1, bf_], [bf_])
                    sc.dma(lambda e, r0=r0, bf_=bf_: e.dma_start(out=base_d[r0:r0 + 128, :], in_=bf_[:]), [bf_], [base_b], q='act')
                stage_X(0)
                for t in range(NT):
                    if t + 1 < NT:
                        stage_X(t + 1)
                    stage_Y(t)
                sc.pop()
        if debug:
            for nm, bb_, o in (('E8', E8, 0), ('W8', W8, 8), ('R8', R8, 16)):
                sc.dma(lambda e, bb_=bb_, o=o: e.dma_start(out=dbg2_d[:, :, o:o + 8], in_=bb_[:]), [bb_], [dbg2_b])
        sc.pop()

        if stop == 'A':
            sc.wait_all('sp', [dbg_b, dbg2_b])
            return nc
        sc.push()
        pad = sc.sb('pad', [128, E], F32)
        pend = sc.sb('pend', [128, E], F32)
        pstart = sc.sb('pstart', [128, E], F32)
        sc.op('dve', lambda e: e.tensor_scalar(out=pad[:], in0=cnt_run[:], scalar1=127.0, scalar2=1.0 / 128.0, op0=ALU.add, op1=ALU.mult), [cnt_run], [pad])
        sc.op('dve', lambda e: e.tensor_scalar(out=pad[:], in0=pad[:], scalar1=-0.49609375, scalar2=8388608.0, op0=ALU.add, op1=ALU.add), [pad], [pad])
        sc.op('dve', lambda e: e.tensor_scalar(out=pad[:], in0=pad[:], scalar1=-8388608.0, scalar2=128.0, op0=ALU.add, op1=ALU.mult), [pad], [pad])
        sc.op('dve', lambda e: e.tensor_tensor_scan(out=pend[:], data0=ones32[:], data1=pad[:], initial=0.0, op0=ALU.mult, op1=ALU.add), [ones32, pad], [pend])
        sc.op('dve', lambda e: e.tensor_tensor(out=pstart[:], in0=pend[:], in1=pad[:], op=ALU.subtract), [pend, pad], [pstart])
        thr = cload('thr', thr_d[:, :], [128, NBLK])
        cmpb = sc.sb('cmpb', [128, 2, NBLK], BF16)
        pcol = sc.sb('pcol', [128, 2], F32)
        for half in range(2):
            ps = psn()
            sc.op('pe', lambda e, ps=ps, half=half: e.transpose(ps[:, 0:128], pend[:, half * 128:(half + 1) * 128], ident[:]), [pend, ident], [ps])
            sc.op('act', lambda e, ps=ps, half=half: e.activation(out=pcol[:, half:half + 1], in_=ps[:, 0:1], func=AF.Identity), [ps], [pcol])
            sc.op('dve', lambda e, half=half: e.tensor_scalar(out=cmpb[:, half, :], in0=thr[:], scalar1=pcol[:, half:half + 1], scalar2=None, op0=ALU.is_ge), [thr, pcol], [cmpb])
        blk_f = sc.sb('blk_f', [128, NBLK], F32)
        widx = sc.sb('widx', [128, NBLK], I32)
        c0 = 0
        while c0 < NBLK:
            cw = min(512, NBLK - c0)
            ps = psn()
            for half in range(2):
                sc.op('pe', lambda e, ps=ps, half=half, c0=c0, cw=cw: e.matmul(ps[:, 0:cw], lhsT=onesb[:, :], rhs=cmpb[:, half, c0:c0 + cw], start=(half == 0), stop=(half == 1)), [onesb, cmpb], [ps], inc=(half == 1))
            sc.op('dve', lambda e, ps=ps, c0=c0, cw=cw: e.tensor_scalar(out=blk_f[:, c0:c0 + cw], in0=ps[:, 0:cw], scalar1=float(E - 1), scalar2=128.0, op0=ALU.min, op1=ALU.mult), [ps], [blk_f])
            c0 += cw
        eqf = sc.sb('eqf', [128, NBLK], F32)
        sc.op('pool', lambda e: e.memset(eqf[:], 0.0), [], [eqf])
        sc.op('dve', lambda e: e.tensor_tensor(out=eqf[:, 2:NBLK], in0=blk_f[:, 2:NBLK], in1=blk_f[:, 0:NBLK - 2], op=ALU.is_equal), [blk_f], [eqf])
        sc.op('dve', lambda e: e.tensor_scalar(out=blk_f[:], in0=blk_f[:], scalar1=piota[:, 0:1], scalar2=None, op0=ALU.add), [blk_f, piota], [blk_f])
        sc.op('dve', lambda e: e.scalar_tensor_tensor(out=blk_f[:], in0=eqf[:], scalar=1.0e9, in1=blk_f[:], op0=ALU.mult, op1=ALU.add), [eqf, blk_f], [blk_f])
        sc.op('dve', lambda e: e.tensor_copy(out=widx[:], in_=blk_f[:]), [blk_f], [widx])
        d8f_l = [sc.sb('d8f%d' % i, [128, 8], F32) for i in range(2)]
        hfl = [sc.sb('hfl%d' % i, [128, D], BF16) for i in range(2)]
        for gt in range(NTT):
            r0 = gt * 128
            d8f = d8f_l[gt % 2]
            for k in range(8):
                sc.op('dve', lambda e, gt=gt, k=k, d8f=d8f: e.scalar_tensor_tensor(out=junk[:, 0:E], in0=iota1[:], scalar=E8[:, gt, k:k + 1], in1=pstart[:], op0=ALU.is_equal, op1=ALU.mult, accum_out=d8f[:, k:k + 1]),
                      [iota1, E8, pstart], [junk, d8f], inc=False)
                sc.flush('dve', scr['dve'])
            sc.op('dve', lambda e, gt=gt, d8f=d8f: e.tensor_tensor(out=d8f[:], in0=d8f[:], in1=R8[:, gt, :], op=ALU.add), [d8f, R8], [d8f])
            sc.op('dve', lambda e, gt=gt, d8f=d8f: e.tensor_copy(out=D8i[gt][:], in_=d8f[:]), [d8f], [D8i[gt]])
            hl = hfl[gt % 2]
            load(hl, hl[:], hfs_d[r0:r0 + 128, :], extra_reads=[hfs_b])
            for k in range(8):
                sc.dma(lambda e, gt=gt, k=k, hl=hl: e.indirect_dma_start(out=xs_d[:, :], out_offset=bass.IndirectOffsetOnAxis(ap=D8i[gt][:, k:k + 1], axis=0), in_=hl[:], in_offset=None),
                       [hl, D8i[gt]], [xs_b], q='pool')
        if debug:
            for g in range(NTT):
                sc.dma(lambda e, g=g: e.dma_start(out=dbg2_d[:, g, 24:32], in_=D8i[g][:].bitcast(F32)), [D8i[g]], [dbg2_b])
            sc.dma(lambda e: e.dma_start(out=dbg3_d[0:1, 0:NBLK], in_=blk_f[0:1, :]), [blk_f], [dbg3_b])
            sc.dma(lambda e: e.dma_start(out=dbg3_d[1:2, 0:E], in_=cnt_run[0:1, :]), [cnt_run], [dbg3_b])
            sc.dma(lambda e: e.dma_start(out=dbg3_d[2:3, 0:E], in_=pstart[0:1, :]), [pstart], [dbg3_b])

        if stop == 'B0':
            sc.wait_all('sp', [dbg_b, dbg2_b, dbg3_b, xs_b])
            return nc
        sc.push()
        xbk = [sc.sb('xbk%d' % i, [128, D], BF16) for i in range(6)]
        xTk = [sc.sb('xTk%d' % i, [128, 8, 128], BF16) for i in range(2)]
        wst = [sc.sb('wst%d' % i, [128, 6144], F32) for i in range(2)]
        wgu = [sc.sb('wgu%d' % i, [128, 8, 512], BF16) for i in range(3)]
        wdb = [sc.sb('wdb%d' % i, [128, 2, D], BF16) for i in range(3)]
        sgb = [sc.sb('sgb%d' % i, [128, 256], F32) for i in range(2)]
        ab = [sc.sb('ab%d' % i, [128, 256], BF16) for i in range(2)]
        aTb = [sc.sb('aTb%d' % i, [128, 2, 128], BF16) for i in range(2)]
        yb = [sc.sb('yb%d' % i, [128, D], BF16) for i in range(2)]
        bc_regs = nc.alloc_registers('bcreg', [mybir.EngineType.Pool])
        nc.regs_mov(bc_regs, E * 128 - 1)
        bc_val = nc.gpsimd.snap(bc_regs.handles[0], donate=True)
        ps_g_l = {}

        def emit_gather(blk):
            if not (0 <= blk < NBLK):
                return
            i = blk % 2
            sc.dma(lambda e, i=i, blk=blk: e.indirect_dma_start(out=wst[i][:, :], out_offset=None, in_=wcat_d[:, :],
                                                                in_offset=bass.IndirectOffsetOnAxis(ap=widx[:, blk:blk + 1], axis=0),
                                                                bounds_check=bc_val, oob_is_err=False),
                   [widx], [wst[i]], q='pool')

        def stage_L(blk):
            if not (0 <= blk < NBLK):
                return
            i6 = blk % 6
            load(xbk[i6], xbk[i6][:], xs_d[blk * 128:(blk + 1) * 128, :], extra_reads=[xs_b])

        def stage_A(blk):
            if not (0 <= blk < NBLK):
                return
            i2 = blk % 2
            i3 = blk % 3
            sc.op('dve', lambda e: e.tensor_copy(out=wgu[i3][:, :, 0:256], in_=wst[i2][:, 0:2048].rearrange("p (j f) -> p j f", j=8)), [wst[i2]], [wgu[i3]])
            sc.op('act', lambda e: e.activation(out=wgu[i3][:, :, 256:512], in_=wst[i2][:, 2048:4096].rearrange("p (j f) -> p j f", j=8), func=AF.Identity), [wst[i2]], [wgu[i3]])
            sc.op('dve', lambda e: e.tensor_copy(out=wdb[i3][:].rearrange("p j n -> p (j n)"), in_=wst[i2][:, 4096:6144]), [wst[i2]], [wdb[i3]])
            ps = psn('c')
            pv = ps[:, :].bitcast(BF16)
            for k in range(8):
                sc.op('pe', lambda e, k=k: e.transpose(pv[:, k * 128:(k + 1) * 128], xbk[blk % 6][:, k:D:8], identb[:]), [xbk[blk % 6], identb], [ps], inc=(k == 7))
            sc.op('act', lambda e: e.activation(out=xTk[i2][:].rearrange("p k n -> p (k n)"), in_=pv, func=AF.Identity), [ps], [xTk[i2]])

        def stage_B(blk):
            if not (0 <= blk < NBLK):
                return
            i2 = blk % 2
            i3 = blk % 3
            ps_g = psn('b')
            for k in range(8):
                sc.op('pe', lambda e, k=k: e.matmul(ps_g[:, :], lhsT=xTk[i2][:, k, :], rhs=wgu[i3][:, k, :], start=(k == 0), stop=(k == 7)), [xTk[i2], wgu[i3]], [ps_g], inc=(k == 7))
            sc.op('act', lambda e: e.activation(out=sgb[i2][:], in_=ps_g[:, 0:256], func=AF.Silu), [ps_g], [sgb[i2]])
            sc.op('dve', lambda e: e.tensor_tensor(out=ab[i2][:], in0=sgb[i2][:], in1=ps_g[:, 256:512], op=ALU.mult), [sgb[i2], ps_g], [ab[i2]])

        def stage_C(blk):
            if not (0 <= blk < NBLK):
                return
            i2 = blk % 2
            i3 = blk % 3
            r0 = blk * 128
            ps_t = psn('c')
            ptv = ps_t[:, :].bitcast(BF16)
            for kk in range(2):
                sc.op('pe', lambda e, kk=kk: e.transpose(ptv[:, kk * 128:(kk + 1) * 128], ab[i2][:, kk:256:2], identb[:]), [ab[i2], identb], [ps_t], inc=(kk == 1))
            sc.op('act', lambda e: e.activation(out=aTb[i2][:].rearrange("p a n -> p (a n)"), in_=ptv[:, 0:256], func=AF.Identity), [ps_t], [aTb[i2]])
            for half in range(2):
                hs = slice(half * 512, (half + 1) * 512)
                ps = psn('a')
                for kk in range(2):
                    sc.op('pe', lambda e, kk=kk, ps=ps, hs=hs: e.matmul(ps[:, :], lhsT=aTb[i2][:, kk, :], rhs=wdb[i3][:, kk, hs], start=(kk == 0), stop=(kk == 1)), [aTb[i2], wdb[i3]], [ps], inc=(kk == 1))
                if half == 0:
                    sc.op('act', lambda e, ps=ps, hs=hs: e.activation(out=yb[i2][:, hs], in_=ps[:, :], func=AF.Identity), [ps], [yb[i2]])
                else:
                    sc.op('dve', lambda e, ps=ps, hs=hs: e.tensor_copy(out=yb[i2][:, hs], in_=ps[:, :]), [ps], [yb[i2]])
            sc.dma(lambda e: e.dma_start(out=ys_d[r0:r0 + 128, :], in_=yb[i2][:]), [yb[i2]], [ys_b], q='act')

        emit_gather(0)
        emit_gather(1)
        for j in range(4):
            stage_L(j)
        stage_A(0)
        emit_gather(2)
        stage_A(1)
        emit_gather(3)
        stage_B(0)
        for it in range(NBLK):
            stage_L(it + 4)
            stage_A(it + 2)
            emit_gather(it + 4)
            stage_B(it + 1)
            stage_C(it)
        sc.pop()

        sc.push()
        lnbc2 = [sc.sb('ln2bc%d' % v, [128, D], F32) for v in range(2)]
        for v in range(2):
            load(lnbc2[v], lnbc2[v][:], ln_d[2 + v:3 + v, :].partition_broadcast(128))
        lnw = {'st': sc.sb('ln2_st', [128, 2, SD], F32), 'mv': sc.sb('ln2_mv', [128, AD], F32),
               'rstd': sc.sb('ln2_rstd', [128, 1], F32), 'nb': sc.sb('ln2_nb', [128, 1], F32)}
        yg = [sc.sb('yg%d' % i, [128, 8, D], BF16) for i in range(2)]
        acc = sc.sb('acc', [128, D], F32)
        dgl = [sc.sb('dg%d' % i, [128, 8, 128], BF16) for i in range(2)]
        bsl = [sc.sb('bsl%d' % i, [128, D], F32) for i in range(2)]
        ot = [sc.sb('ot%d' % i, [128, D], F32) for i in range(2)]
        for gt in range(NTT):
            b = gt // NT
            r0 = gt * 128
            ygb = yg[gt % 2]
            for k in range(8):
                sc.dma(lambda e, gt=gt, k=k, ygb=ygb: e.indirect_dma_start(out=ygb[:, k, :], out_offset=None, in_=ys_d[:, :], in_offset=bass.IndirectOffsetOnAxis(ap=D8i[gt][:, k:k + 1], axis=0)),
                       [ys_b, D8i[gt]], [ygb], q='pool')
            bs_ = bsl[gt % 2]
            load(bs_, bs_[:], base_d[r0:r0 + 128, :], extra_reads=[base_b])
            dg = dgl[gt % 2]
            sc.op('dve', lambda e, gt=gt, dg=dg: e.tensor_tensor(out=dg[:], in0=identb[:].unsqueeze(1).to_broadcast([128, 8, 128]), in1=W8[:, gt, :].unsqueeze(2).to_broadcast([128, 8, 128]), op=ALU.mult),
                  [identb, W8], [dg])
            ps_c = [psn('a'), psn('a')]
            for half in range(2):
                hs = slice(half * 512, (half + 1) * 512)
                for k in range(8):
                    sc.op('pe', lambda e, k=k, half=half, hs=hs, dg=dg, ygb=ygb, ps_c=ps_c: e.matmul(ps_c[half][:, :], lhsT=dg[:, k, :], rhs=ygb[:, k, hs], start=(k == 0), stop=(k == 7)),
                          [dg, ygb], [ps_c[half]], inc=(k == 7))
                sc.op('dve', lambda e, half=half, hs=hs, b=b, ps_c=ps_c: e.tensor_tensor(out=acc[:, hs], in0=ps_c[half][:, :], in1=gf_all[:, b, hs], op=ALU.mult), [ps_c[half], gf_all], [acc])
            sc.op('dve', lambda e, bs_=bs_: e.tensor_tensor(out=acc[:], in0=acc[:], in1=bs_[:], op=ALU.add), [acc, bs_], [acc])
            o_ = ot[gt % 2]
            ln_tile(acc, o_, lnbc2[0], lnbc2[1], eps_ln, 'ln2')
            sc.dma(lambda e, r0=r0, o_=o_: e.dma_start(out=out_d[r0:r0 + 128, :], in_=o_[:]), [o_], [out_b], q='act')
        fin = [out_b] + ([dbg_b, dbg2_b, dbg3_b] if debug else [])
        sc.wait_all('sp', fin)
        sc.pop()
        sc.pop()
    return nc


def make_in_maps(inputs, nseq, ncores):
    x = np.ascontiguousarray(inputs['x'], dtype=np.float32)
    c = np.asarray(inputs['c'], dtype=np.float32)
    rpb = np.asarray(inputs['na_rpb'], dtype=np.float32)[0]
    consts = _host_consts(rpb)
    nblk = nblk_for(nseq)
    thr = np.broadcast_to((128.0 * np.arange(nblk, dtype=np.float32))[None, :], (128, nblk)).copy()
    shared = {
        'w_ada': np.ascontiguousarray(inputs['w_ada'][0]), 'b_ada': np.ascontiguousarray(inputs['b_ada'][0][None, :]),
        'w_in': np.ascontiguousarray(inputs['w_in'][0]), 'w_out': np.ascontiguousarray(inputs['w_out'][0]),
        'biasT': consts['biasT'], 'lg': np.ascontiguousarray(inputs['ret_log_decay'][0].reshape(1, 8)),
        'gn_g': np.ascontiguousarray(inputs['ret_gn_g'][0][None, :]),
        'ln': np.ascontiguousarray(np.stack([inputs['ln1_g'][0], inputs['ln1_b'][0], inputs['ln2_g'][0], inputs['ln2_b'][0]], 0)),
        'w_router': np.ascontiguousarray(inputs['w_router'][0]), 'router_bias': np.ascontiguousarray(inputs['router_bias'][0][None, :]),
        'wcat': np.concatenate([np.asarray(inputs['w_gate'][0], np.float32).reshape(E, 128, 2048), np.asarray(inputs['w_up'][0], np.float32).reshape(E, 128, 2048),
                                np.asarray(inputs['w_down'][0], np.float32).reshape(E, 128, 2048)], axis=2).reshape(E * 128, 6144),
        'ws_gate': np.ascontiguousarray(inputs['ws_gate'][0]), 'ws_up': np.ascontiguousarray(inputs['ws_up'][0]), 'ws_down': np.ascontiguousarray(inputs['ws_down'][0]),
        'thrrow': thr,
    }
    for k in ('ident', 'lstrict', 'iota1', 'dpos', 'dneg', 'maskf', 'maskb', 'ifp1', 'ifm', 'pexp', 'cos', 'sin', 'nsin'):
        shared[k] = consts[k]
    shared = {k: np.ascontiguousarray(v, dtype=np.float32) for k, v in shared.items()}
    maps = []
    for ci in range(ncores):
        m = dict(shared)
        m['x'] = np.ascontiguousarray(x[ci * nseq:(ci + 1) * nseq].reshape(nseq * S, D))
        c4 = c[ci * nseq:(ci + 1) * nseq]
        m['cT'] = np.ascontiguousarray(c4.T.reshape(8, 128, nseq).transpose(1, 0, 2))
        maps.append(m)
    return maps


def kernel(**inputs):
    nseq = inputs['x'].shape[0] // NCORES
    nc = build_nc(nseq)
    maps = make_in_maps(inputs, nseq, NCORES)
    res = run_bass_kernel_spmd(nc, maps, core_ids=list(range(NCORES)))
    outs = [r['out'].reshape(nseq, S, D) for r in res.results]
    return np.concatenate(outs, axis=0).astype(np.float32)
```
